# Optimizing a Trainium2 kernel written in Bass

```python
import jax, jax.numpy as jnp
from jax import lax
import numpy as np

D_MODEL = 1024
BATCH = 2
SEQ = 16384
DEPTH = 4

N_META = 16
DEEPNORM_ALPHA = (2 * DEPTH) ** 0.25
DEEPNORM_BETA = (8 * DEPTH) ** -0.25
LN_EPS = 1e-5
RG_WIDTH = 1344
RG_BLOCKS = 8
RG_BLOCK_W = RG_WIDTH // RG_BLOCKS
RG_CONV_W = 4
RG_C = 8.0
GLA_HEADS = 4
GLA_DK = D_MODEL // 2 // GLA_HEADS
GLA_DV = D_MODEL // GLA_HEADS
GLA_QK = GLA_HEADS * GLA_DK
GLA_VD = GLA_HEADS * GLA_DV
GLA_RANK = 16
GLA_TAU = 16.0
GLA_CHUNK = 64
GLA_PAD = (-N_META) % GLA_CHUNK
D_FF = 3584
N_EXPERTS = 8
TOP_K = 2
N_REC = (DEPTH + 1) // 2
N_GLA = DEPTH // 2

kernel_name = "hybrid_rglru_gla_moe_deepnorm"


def _normalize(x):
    xf = x.astype(jnp.float32)
    mu = jnp.mean(xf, axis=-1, keepdims=True)
    xc = xf - mu
    var = jnp.mean(xc * xc, axis=-1, keepdims=True)
    return xc * lax.rsqrt(var + LN_EPS)


def layer_norm(x, g, b):
    return (_normalize(x) * g + b).astype(x.dtype)


def swiglu(h, w_in, w_out):
    gate, up = jnp.split(h @ w_in, 2, axis=-1)
    return (jax.nn.silu(gate) * up) @ w_out


def _linear_scan_combine(c1, c2):
    a1, b1 = c1
    a2, b2 = c2
    return a1 * a2, a2 * b1 + b2


def rglru_block(h, w_in, conv_w, conv_b, w_gates, b_gates, lam, w_out):
    Bsz, T, _ = h.shape
    proj = h @ w_in
    gate_branch, xr = jnp.split(proj, 2, axis=-1)
    xp = jnp.pad(xr, ((0, 0), (RG_CONV_W - 1, 0), (0, 0)))
    xc = conv_b
    for tap in range(RG_CONV_W):
        xc = xc + xp[:, tap:tap + T] * conv_w[tap]
    gates = jnp.einsum('btnc,gncd->gbtnd', xc.reshape(Bsz, T, RG_BLOCKS, RG_BLOCK_W), w_gates)
    gates = gates.reshape(2, Bsz, T, RG_WIDTH) + b_gates[:, None, None, :]
    gates = jax.nn.sigmoid(gates.astype(jnp.float32))
    r_t, i_t = gates[0], gates[1]
    log_a = -RG_C * r_t * jax.nn.softplus(-lam.astype(jnp.float32))
    a_t = jnp.exp(log_a)
    u_t = jnp.sqrt(-jnp.expm1(2.0 * log_a)) * (i_t * xc)
    _, hs = lax.associative_scan(_linear_scan_combine, (a_t, u_t), axis=1)
    y = hs * jax.nn.gelu(gate_branch.astype(jnp.float32))
    return (y @ w_out).astype(h.dtype)


def gla_block(h, w_in, w_gate_up, b_gate, norm_g, w_out):
    Bsz, T, _ = h.shape
    proj = h @ w_in
    q, k, v, r, z = jnp.split(
        proj, [GLA_QK, 2 * GLA_QK, 2 * GLA_QK + GLA_VD, 2 * GLA_QK + 2 * GLA_VD], axis=-1)
    log_g = jax.nn.log_sigmoid((z @ w_gate_up + b_gate).astype(jnp.float32)) / GLA_TAU
    L = T + GLA_PAD
    nc = L // GLA_CHUNK

    def to_chunks(t, d):
        t = jnp.pad(t, ((0, 0), (GLA_PAD, 0), (0, 0)))
        return t.reshape(Bsz, nc, GLA_CHUNK, GLA_HEADS, d).transpose(0, 3, 1, 2, 4)

    qc = to_chunks(q, GLA_DK) * (GLA_DK ** -0.5)
    kc = to_chunks(k, GLA_DK)
    vc = to_chunks(v, GLA_DV)
    gc = to_chunks(log_g, GLA_DK)
    b = jnp.cumsum(gc, axis=3)
    b_mid = b[:, :, :, GLA_CHUNK // 2 - 1:GLA_CHUNK // 2]
    b_last = b[:, :, :, -1:]
    scores = jnp.einsum('bhncd,bhnsd->bhncs', qc * jnp.exp(b - b_mid), kc * jnp.exp(b_mid - b))
    causal = jnp.tril(jnp.ones((GLA_CHUNK, GLA_CHUNK), dtype=bool))
    scores = jnp.where(causal, scores, 0.0)
    o_intra = jnp.einsum('bhncs,bhnse->bhnce', scores, vc)
    q_in = qc * jnp.exp(b)
    k_out = kc * jnp.exp(b_last - b)
    decay = jnp.exp(b_last[:, :, :, 0])

    def step(S, xs):
        qn, kn, vn, dn = xs
        o = jnp.einsum('bhcd,bhde->bhce', qn, S)
        S = dn[..., None] * S + jnp.einsum('bhcd,bhce->bhde', kn, vn)
        return S, o

    S0 = jnp.zeros((Bsz, GLA_HEADS, GLA_DK, GLA_DV), jnp.float32)
    xs = (jnp.moveaxis(q_in, 2, 0), jnp.moveaxis(k_out, 2, 0),
          jnp.moveaxis(vc, 2, 0), jnp.moveaxis(decay, 2, 0))
    _, o_inter = lax.scan(step, S0, xs)
    o = o_intra + jnp.moveaxis(o_inter, 0, 2)
    o = o.transpose(0, 2, 3, 1, 4).reshape(Bsz, L, GLA_HEADS, GLA_DV)[:, GLA_PAD:]
    o = _normalize(o) * norm_g
    o = o.reshape(Bsz, T, GLA_VD) * jax.nn.silu(r.astype(jnp.float32))
    return (o @ w_out).astype(h.dtype)


def moe_swiglu(h, router, w_in, w_out):
    Bsz, T, D = h.shape
    hf = h.reshape(Bsz * T, D)
    logits = (hf @ router).astype(jnp.float32)
    top_logit, top_idx = lax.top_k(logits, TOP_K)
    top_w = jax.nn.softmax(top_logit, axis=-1)
    gates = jnp.einsum('nk,nke->ne', top_w, jax.nn.one_hot(top_idx, N_EXPERTS, dtype=jnp.float32))
    y = jnp.zeros((Bsz * T, D), jnp.float32)
    for e in range(N_EXPERTS):
        y = y + gates[:, e:e + 1] * swiglu(hf, w_in[e], w_out[e])
    return y.reshape(Bsz, T, D).astype(h.dtype)


def setup_inputs(seed: int = 0) -> dict:
    key = jax.random.key(seed)
    ks = jax.random.split(key, 24)
    f32 = jnp.float32

    def nrm(k, shape, fan_in, scale=1.0):
        return jax.random.normal(k, shape, f32) * (scale * fan_in ** -0.5)

    x = jax.random.normal(ks[0], (BATCH, SEQ, D_MODEL), f32)
    meta_tokens = jax.random.normal(ks[1], (N_META, D_MODEL), f32)
    ln_gain = 1.0 + 0.02 * jax.random.normal(ks[2], (DEPTH, 2, D_MODEL), f32)
    ln_bias = 0.02 * jax.random.normal(ks[3], (DEPTH, 2, D_MODEL), f32)
    rg_w_in = nrm(ks[4], (N_REC, D_MODEL, 2 * RG_WIDTH), D_MODEL)
    rg_conv_w = nrm(ks[5], (N_REC, RG_CONV_W, RG_WIDTH), RG_CONV_W)
    rg_conv_b = 0.02 * jax.random.normal(ks[6], (N_REC, RG_WIDTH), f32)
    rg_w_gates = nrm(ks[7], (N_REC, 2, RG_BLOCKS, RG_BLOCK_W, RG_BLOCK_W), RG_BLOCK_W)
    rg_b_gates = 0.02 * jax.random.normal(ks[8], (N_REC, 2, RG_WIDTH), f32)
    a_pow_c = jax.random.uniform(ks[9], (N_REC, RG_WIDTH), f32, minval=0.9, maxval=0.999)
    a_base = a_pow_c ** (1.0 / RG_C)
    rg_lambda = jnp.log(a_base) - jnp.log1p(-a_base)
    rg_w_out = nrm(ks[10], (N_REC, RG_WIDTH, D_MODEL), RG_WIDTH, DEEPNORM_BETA)
    gla_w_in = nrm(ks[11], (N_GLA, D_MODEL, 2 * GLA_QK + 2 * GLA_VD + GLA_RANK), D_MODEL)
    gla_w_gate_up = nrm(ks[12], (N_GLA, GLA_RANK, GLA_QK), GLA_RANK)
    gla_b_gate = 2.0 + 0.1 * jax.random.normal(ks[13], (N_GLA, GLA_QK), f32)
    gla_norm_g = 1.0 + 0.02 * jax.random.normal(ks[14], (N_GLA, GLA_HEADS, GLA_DV), f32)
    gla_w_out = nrm(ks[15], (N_GLA, GLA_VD, D_MODEL), GLA_VD, DEEPNORM_BETA)
    ffn_w_in = nrm(ks[16], (N_REC, D_MODEL, 2 * D_FF), D_MODEL)
    ffn_w_out = nrm(ks[17], (N_REC, D_FF, D_MODEL), D_FF, DEEPNORM_BETA)
    moe_router = nrm(ks[18], (N_GLA, D_MODEL, N_EXPERTS), D_MODEL)
    moe_w_in = nrm(ks[19], (N_GLA, N_EXPERTS, D_MODEL, 2 * D_FF), D_MODEL)
    moe_w_out = nrm(ks[20], (N_GLA, N_EXPERTS, D_FF, D_MODEL), D_FF, DEEPNORM_BETA)
    return {
        "x": x, "meta_tokens": meta_tokens, "ln_gain": ln_gain, "ln_bias": ln_bias,
        "rg_w_in": rg_w_in, "rg_conv_w": rg_conv_w, "rg_conv_b": rg_conv_b,
        "rg_w_gates": rg_w_gates, "rg_b_gates": rg_b_gates, "rg_lambda": rg_lambda,
        "rg_w_out": rg_w_out,
        "gla_w_in": gla_w_in, "gla_w_gate_up": gla_w_gate_up, "gla_b_gate": gla_b_gate,
        "gla_norm_g": gla_norm_g, "gla_w_out": gla_w_out,
        "ffn_w_in": ffn_w_in, "ffn_w_out": ffn_w_out,
        "moe_router": moe_router, "moe_w_in": moe_w_in, "moe_w_out": moe_w_out,
    }


def reference(x, meta_tokens, ln_gain, ln_bias,
              rg_w_in, rg_conv_w, rg_conv_b, rg_w_gates, rg_b_gates, rg_lambda, rg_w_out,
              gla_w_in, gla_w_gate_up, gla_b_gate, gla_norm_g, gla_w_out,
              ffn_w_in, ffn_w_out, moe_router, moe_w_in, moe_w_out):
    Bsz = x.shape[0]
    meta = jnp.broadcast_to(meta_tokens.astype(x.dtype)[None], (Bsz, N_META, D_MODEL))
    h = jnp.concatenate([meta, x], axis=1)
    for i in range(DEPTH):
        j = i // 2
        if i % 2 == 0:
            mix = rglru_block(h, rg_w_in[j], rg_conv_w[j], rg_conv_b[j], rg_w_gates[j],
                              rg_b_gates[j], rg_lambda[j], rg_w_out[j])
        else:
            mix = gla_block(h, gla_w_in[j], gla_w_gate_up[j], gla_b_gate[j],
                            gla_norm_g[j], gla_w_out[j])
        h = layer_norm(DEEPNORM_ALPHA * h + mix, ln_gain[i, 0], ln_bias[i, 0])
        if i % 2 == 0:
            ff = swiglu(h, ffn_w_in[j], ffn_w_out[j]).astype(h.dtype)
        else:
            ff = moe_swiglu(h, moe_router[j], moe_w_in[j], moe_w_out[j])
        h = layer_norm(DEEPNORM_ALPHA * h + ff, ln_gain[i, 1], ln_bias[i, 1])
    return h[:, N_META:]
```

```python
import numpy as np
from contextlib import ExitStack
import concourse.bass as bass
import concourse.mybir as mybir
from concourse.bass_utils import run_bass_kernel_spmd

F32 = mybir.dt.float32
BF16 = mybir.dt.bfloat16
AF = mybir.ActivationFunctionType
ALU = mybir.AluOpType
AX = mybir.AxisListType

D = 1024
NCORE = 8
SEG = 4096
NMETA = 16
NTOK = SEG + NMETA
DEPTH = 4
ALPHA = float((2 * DEPTH) ** 0.25)
LN_EPS = 1e-5
RGW = 1344
CT = 84
NCT = 16
DFF = 3584
NEXP = 8
GQK = 512
GVD = 1024
GIN = 3088
NDMASEM = 8

ENG_ATTR = {"pe": "tensor", "act": "scalar", "dve": "vector", "pool": "gpsimd", "sp": "sync"}


class TK:
    __slots__ = ("lw", "rd")

    def __init__(self):
        self.lw = None
        self.rd = []


class Buf:
    def __init__(self, t, ntk=1):
        self.t = t
        self.tks = [TK() for _ in range(ntk)]

    def __getitem__(self, idx):
        return self.t[idx]

    @property
    def tk(self):
        return self.tks[0]


class Glob:
    def __init__(self, nc, stack):
        self.nc = nc
        self.sem = {}
        self.cnt = {}
        for e in ("pe", "act", "dve", "pool"):
            self.sem[e] = stack.enter_context(nc.semaphore("s_" + e))
            self.cnt[e] = 0
        self.dsem = {}
        self.dcnt = {}
        for q in ("sp", "pool"):
            self.dsem[q] = [stack.enter_context(nc.semaphore("d_%s%d" % (q, i))) for i in range(NDMASEM)]
            self.dcnt[q] = 0
        self.ccsem = [stack.enter_context(nc.semaphore("cc%d" % i)) for i in range(28)]
        self.ncc = 0
        allsems = list(self.sem.values()) + self.dsem["sp"] + self.dsem["pool"] + self.ccsem
        with nc.Block() as block:
            @block.gpsimd
            def _(e):
                for s_ in allsems:
                    e.sem_clear(s_)
        self.nphase = 0
        self.dram = {}

    def dt(self, name, shape, dtype):
        if name not in self.dram:
            self.dram[name] = self.nc.dram_tensor(name, list(shape), dtype)
        return self.dram[name]


class Phase:
    def __init__(self, G, name):
        self.G = G
        self.nc = G.nc
        self.name = "%s_%d" % (name, G.nphase)
        G.nphase += 1
        self.ops = []
        self.stack = ExitStack()
        self.nbuf = 0
        self.dtk = {}
        self.dma_hist = {"sp": [], "pool": []}

    def sb(self, shape, dtype=F32, ntk=1, name=None):
        self.nbuf += 1
        t = self.stack.enter_context(self.nc.sbuf_tensor("%s_b%d" % (self.name, self.nbuf), list(shape), dtype))
        return Buf(t, ntk)

    def ps(self, shape, dtype=F32, ntk=1):
        self.nbuf += 1
        t = self.stack.enter_context(self.nc.psum_tensor("%s_p%d" % (self.name, self.nbuf), list(shape), dtype))
        return Buf(t, ntk)

    def dk(self, *key):
        if key not in self.dtk:
            self.dtk[key] = TK()
        return self.dtk[key]

    def op(self, eng, fn, w=(), r=(), kind="c"):
        idx = len(self.ops)
        deps = set()
        for tk in r:
            if tk.lw is not None:
                deps.add(tk.lw)
        for tk in w:
            if tk.lw is not None:
                deps.add(tk.lw)
            deps.update(tk.rd)
        keep = []
        for d in deps:
            o = self.ops[d]
            if kind == "c" and o["kind"] == "c" and o["eng"] == eng:
                if eng == "pe":
                    continue
                israw = any(tk.lw == d for tk in r)
                if not israw:
                    continue
            keep.append(d)
        if kind == "d":
            h = self.dma_hist[eng]
            if len(h) >= NDMASEM:
                keep.append(h[-NDMASEM])
            h.append(idx)
        self.ops.append({"eng": eng, "fn": fn, "deps": sorted(set(keep)), "kind": kind, "needed": kind != "c"})
        for tk in r:
            if kind == "c":
                tk.rd = [x for x in tk.rd if not (self.ops[x]["kind"] == "c" and self.ops[x]["eng"] == eng)]
            tk.rd.append(idx)
        for tk in w:
            tk.lw = idx
            tk.rd = []
        return idx

    def pe(self, fn, w=(), r=()):
        return self.op("pe", fn, w, r)

    def act(self, fn, w=(), r=()):
        return self.op("act", fn, w, r)

    def dve(self, fn, w=(), r=()):
        return self.op("dve", fn, w, r)

    def dma(self, q, out, in_, w=(), r=()):
        return self.op(q, lambda e: e.dma_start(out=out, in_=in_), w, r, kind="d")

    def run(self):
        G = self.G
        ops = self.ops
        tail = []
        for q in ("sp", "pool"):
            tail += self.dma_hist[q][-NDMASEM:]
        ops.append({"eng": "sp", "fn": None, "deps": sorted(tail), "kind": "j", "needed": False})
        for o in ops:
            for d in o["deps"]:
                ops[d]["needed"] = True
        for o in ops:
            if not o["needed"]:
                continue
            if o["kind"] == "c":
                G.cnt[o["eng"]] += 1
                o["tick"] = (G.sem[o["eng"]], G.cnt[o["eng"]], 1)
            elif o["kind"] == "d":
                q = o["eng"]
                i = G.dcnt[q]
                G.dcnt[q] += 1
                o["tick"] = (G.dsem[q][i % NDMASEM], 16 * (i // NDMASEM + 1), 16)
            elif o["kind"] == "cc":
                s = G.ccsem[G.ncc]
                G.ncc += 1
                o["tick"] = (s, 1, 1)
        waited = G.__dict__.setdefault("waited", {})

        def emit(engname, e):
            wd = waited.setdefault(engname, {})
            for o in ops:
                if o["eng"] != engname:
                    continue
                for d in o["deps"]:
                    s, v, _ = ops[d]["tick"]
                    key = id(s)
                    if wd.get(key, 0) < v:
                        e.wait_ge(s, v)
                        wd[key] = v
                if o["fn"] is None:
                    continue
                ins = o["fn"](e)
                if o["needed"]:
                    s, v, inc = o["tick"]
                    ins.then_inc(s, inc)

        with self.nc.Block() as block:
            @block.tensor
            def _(e):
                emit("pe", e)

            @block.scalar
            def _(e):
                emit("act", e)

            @block.vector
            def _(e):
                emit("dve", e)

            @block.gpsimd
            def _(e):
                emit("pool", e)

            @block.sync
            def _(e):
                emit("sp", e)
        self.stack.close()


def subtiles():
    out = []
    for s in range(32):
        out.append((s * 128, 128, s // 4, (s % 4) * 128))
    out.append((SEG, NMETA, 8, 0))
    return out


class LNTail:
    def __init__(self, P, K, li, si, final=False, router=None):
        self.P = P
        self.K = K
        self.final = final
        self.router = router
        nc = P.nc
        self.ident = P.sb([128, 128], F32)
        P.dma("sp", self.ident[:, :], K["ident"][:, :], w=[self.ident.tk])
        self.gb = P.sb([128, D], F32)
        self.bb = P.sb([128, D], F32)
        g = K["ln_gain"][li * 2 + si, :].partition_broadcast(128)
        b = K["ln_bias"][li * 2 + si, :].partition_broadcast(128)
        P.dma("sp", self.gb[:, :], g, w=[self.gb.tk])
        P.dma("sp", self.bb[:, :], b, w=[self.bb.tk])
        self.xn = [P.sb([128, D], F32) for _ in range(2)]
        self.st6 = [P.sb([128, 12], F32) for _ in range(2)]
        self.mv = [P.sb([128, 2], F32) for _ in range(2)]
        self.rs = [P.sb([128, 1], F32) for _ in range(2)]
        self.pst = [P.ps([128, 1024], F32) for _ in range(1)]
        self.hto = [P.sb([128, 8, 512], BF16) for _ in range(2)]
        self.cnt = 0
        self.htcnt = 0
        if router is not None:
            self.rw = P.sb([128, 8, NEXP], F32)
            P.dma("sp", self.rw[:, :, :], router.rearrange("(kc p) e -> p kc e", p=128), w=[self.rw.tk])
            self.ht32 = [P.sb([128, 8, 128], F32) for _ in range(2)]
            for b_ in self.ht32:
                P.dve(lambda e, b_=b_: e.memset(b_[:, :, :], 0.0), w=[b_.tk])
            self.psr = P.ps([128, 512], F32)
            self.gsm = [[P.sb([128, NEXP], F32) for _ in range(4)] for _ in range(2)]
            self.g1 = [[P.sb([128, 1], F32) for _ in range(4)] for _ in range(2)]

    def run(self, acc, acc_tk, sub):
        P, K = self.P, self.K
        row0, rows, httile, col0 = sub
        i = self.cnt % 2
        self.cnt += 1
        xn, st6, mv, rs = self.xn[i], self.st6[i], self.mv[i], self.rs[i]
        for h in range(2):
            P.dve(lambda e, h=h: e.bn_stats(out=st6[:rows, 6 * h:6 * h + 6], in_=acc[:, 512 * h:512 * h + 512]),
                  w=[st6.tk], r=[acc_tk])
        P.dve(lambda e: e.bn_aggr(out=mv[:rows, :], in_=st6[:rows, :]), w=[mv.tk], r=[st6.tk])
        P.dve(lambda e: e.tensor_scalar(out=rs[:rows, :], in0=mv[:rows, 1:2], scalar1=LN_EPS, scalar2=None, op0=ALU.add),
              w=[rs.tk], r=[mv.tk])
        P.act(lambda e: e.activation(out=rs[:rows, :], in_=rs[:rows, :], func=AF.Sqrt), w=[rs.tk], r=[rs.tk])
        P.dve(lambda e: e.reciprocal(out=rs[:rows, :], in_=rs[:rows, :]), w=[rs.tk], r=[rs.tk])
        P.dve(lambda e: e.tensor_scalar(out=xn[:rows, :], in0=acc, scalar1=mv[:rows, 0:1], scalar2=rs[:rows, 0:1],
                                        op0=ALU.subtract, op1=ALU.mult), w=[xn.tk], r=[acc_tk, mv.tk, rs.tk])
        P.dve(lambda e: e.tensor_tensor(out=xn[:rows, :], in0=xn[:rows, :], in1=self.gb[:rows, :], op=ALU.mult),
              w=[xn.tk], r=[xn.tk, self.gb.tk])
        P.dve(lambda e: e.tensor_tensor(out=xn[:rows, :], in0=xn[:rows, :], in1=self.bb[:rows, :], op=ALU.add),
              w=[xn.tk], r=[xn.tk, self.bb.tk])
        if self.final:
            if rows == 128:
                P.dma("sp", K["out"][row0:row0 + rows, :], xn[:rows, :], w=[P.dk("out", row0)], r=[xn.tk])
            return
        P.dma("sp", K["R"][row0:row0 + rows, :], xn[:rows, :], w=[P.dk("R", row0)], r=[xn.tk])
        self.transpose(xn, xn.tk, sub)

    def transpose(self, src, src_tk, sub, rowsel=None):
        P, K = self.P, self.K
        row0, rows, httile, col0 = sub
        pst = self.pst[0]
        for j in range(8):
            P.pe(lambda e, j=j: e.transpose(out=pst[:, j * 128:j * 128 + rows], in_=src[:rows, j * 128:(j + 1) * 128],
                                            identity=self.ident[:rows, :rows]),
                 w=[pst.tk], r=[src_tk, self.ident.tk])
        hto = self.hto[self.htcnt % 2]
        pv = pst[:, :].rearrange("p (j c) -> p j c", j=8)[:, :, 0:rows]
        P.act(lambda e: e.copy(out=hto[:, :, col0:col0 + rows], in_=pv), w=[hto.tk], r=[pst.tk])
        if self.router is not None:
            self.route(pst, sub)
        if col0 + rows == 512 or rows != 128:
            w = 512 if rows == 128 else rows
            P.dma("sp", K["HT"][httile, :, :].rearrange("p (j c) -> p j c", j=8)[:, :, 0:w], hto[:, :, 0:w],
                  w=[P.dk("HT", httile)], r=[hto.tk])
            self.htcnt += 1

    def route(self, pst, sub):
        P, K = self.P, self.K
        row0, rows, httile, col0 = sub
        i = self.cnt % 2
        h32 = self.ht32[i]
        pv = pst[:, :].rearrange("p (j c) -> p j c", j=8)[:, :, 0:rows]
        P.act(lambda e: e.copy(out=h32[:, :, 0:rows], in_=pv), w=[h32.tk], r=[pst.tk])
        psr = self.psr
        for j in range(8):
            P.pe(lambda e, j=j: e.matmul(psr[:, 0:NEXP], lhsT=h32[:, j, :], rhs=self.rw[:, j, :],
                                         start=(j == 0), stop=(j == 7)), w=[psr.tk], r=[h32.tk, self.rw.tk])
        lg, eq, l2, gt = self.gsm[i]
        m1, m2, sm, _ = self.g1[i]
        P.dve(lambda e: e.tensor_copy(out=lg[:rows, :], in_=psr[:rows, 0:NEXP]), w=[lg.tk], r=[psr.tk])
        P.dve(lambda e: e.reduce_max(out=m1[:rows, :], in_=lg[:rows, :], axis=AX.X), w=[m1.tk], r=[lg.tk])
        P.dve(lambda e: e.tensor_scalar(out=eq[:rows, :], in0=lg[:rows, :], scalar1=m1[:rows, 0:1], scalar2=-1e30,
                                        op0=ALU.is_equal, op1=ALU.mult), w=[eq.tk], r=[lg.tk, m1.tk])
        P.dve(lambda e: e.tensor_tensor(out=l2[:rows, :], in0=eq[:rows, :], in1=lg[:rows, :], op=ALU.add),
              w=[l2.tk], r=[eq.tk, lg.tk])
        P.dve(lambda e: e.reduce_max(out=m2[:rows, :], in_=l2[:rows, :], axis=AX.X), w=[m2.tk], r=[l2.tk])
        P.dve(lambda e: e.tensor_scalar(out=eq[:rows, :], in0=lg[:rows, :], scalar1=m2[:rows, 0:1], scalar2=None,
                                        op0=ALU.is_ge), w=[eq.tk], r=[lg.tk, m2.tk, l2.tk])
        P.dve(lambda e: e.tensor_scalar(out=l2[:rows, :], in0=lg[:rows, :], scalar1=m1[:rows, 0:1], scalar2=None,
                                        op0=ALU.subtract), w=[l2.tk], r=[lg.tk, m1.tk])
        P.act(lambda e: e.activation(out=l2[:rows, :], in_=l2[:rows, :], func=AF.Exp), w=[l2.tk], r=[l2.tk])
        P.dve(lambda e: e.tensor_tensor(out=gt[:rows, :], in0=l2[:rows, :], in1=eq[:rows, :], op=ALU.mult),
              w=[gt.tk], r=[l2.tk, eq.tk])
        P.dve(lambda e: e.reduce_sum(out=sm[:rows, :], in_=gt[:rows, :], axis=AX.X), w=[sm.tk], r=[gt.tk])
        P.dve(lambda e: e.reciprocal(out=sm[:rows, :], in_=sm[:rows, :]), w=[sm.tk], r=[sm.tk])
        P.dve(lambda e: e.tensor_scalar(out=gt[:rows, :], in0=gt[:rows, :], scalar1=sm[:rows, 0:1], scalar2=None,
                                        op0=ALU.mult), w=[gt.tk], r=[gt.tk, sm.tk])
        P.dma("sp", K["GT"][row0:row0 + rows, :], gt[:rows, :], w=[P.dk("GT", row0)], r=[gt.tk])


def bc_rows(ap, n):
    return ap.broadcast(0, n)


def phase_prep(G, K):
    P = Phase(G, "prep")
    P.dma("sp", K["R"][0:SEG, :], K["x"][:, :], w=[P.dk("Rall")])
    P.dma("sp", K["R"][SEG:NTOK, :], K["meta"][:, :], w=[P.dk("Rall")])
    T = LNTail.__new__(LNTail)
    T.P, T.K, T.final, T.router = P, K, False, None
    T.ident = P.sb([128, 128], F32)
    P.dma("sp", T.ident[:, :], K["ident"][:, :], w=[T.ident.tk])
    T.pst = [P.ps([128, 1024], F32)]
    T.hto = [P.sb([128, 8, 512], BF16) for _ in range(2)]
    T.htcnt = 0
    T.cnt = 0
    xb = [P.sb([128, D], F32) for _ in range(3)]
    for si, sub in enumerate(subtiles()):
        row0, rows, httile, col0 = sub
        b = xb[si % 3]
        src = K["x"][row0:row0 + rows, :] if rows == 128 else K["meta"][:, :]
        P.dma("sp", b[:rows, :], src, w=[b.tk])
        T.transpose(b, b.tk, sub)
    P.run()


def allgather(P, K, name, src_ap, src_tk, p, c):
    G = P.G
    xin = G.dt("xin_" + name, [p, c], F32)
    xout = G.dt("xout_" + name, [NCORE * p, c], F32)
    P.dma("pool", xin[:, :], src_ap, w=[P.dk("xin", name)], r=[src_tk])
    P.op("pool", lambda e: e.collective_compute("AllGather", ALU.bypass, replica_groups=[list(range(NCORE))],
                                                ins=[xin.ap().opt()], outs=[xout.ap().opt()]),
         w=[P.dk("xout", name)], r=[P.dk("xin", name)], kind="cc")
    g = P.sb([p, NCORE, c], F32)
    P.dma("pool", g[:, :, :], xout[:, :].rearrange("(r p) c -> p r c", p=p), w=[g.tk], r=[P.dk("xout", name)])
    return g


def xch_load(P, K, key, p, c, q="sp"):
    g = P.sb([p, NCORE, c], F32)
    P.dma(q, g[:, :, :], K[key][:, :].rearrange("(r p) c -> p r c", p=p), w=[g.tk], r=[P.dk(key)])
    return g


def phase_cc(G, K, src, dst):
    P = Phase(G, "cc")
    xin, xout = K[src], K[dst]
    P.op("pool", lambda e: e.collective_compute("AllGather", ALU.bypass, replica_groups=[list(range(NCORE))],
                                                ins=[xin.ap().opt()], outs=[xout.ap().opt()]),
         w=[P.dk(dst)], r=[P.dk(src)], kind="cc")
    P.run()


def phase_halo_out(G, K):
    P = Phase(G, "halo")
    hl = P.sb([128, 8, 3], BF16)
    P.dma("sp", hl[:, :, :], K["HT"][7, :, :].rearrange("p (j c) -> p j c", j=8)[:, :, 509:512], w=[hl.tk], r=[P.dk("HT", 7)])
    hlf = P.sb([128, 24], F32)
    P.dve(lambda e: e.tensor_copy(out=hlf[:, :].rearrange("p (j c) -> p j c", j=8), in_=hl[:, :, :]), w=[hlf.tk], r=[hl.tk])
    P.dma("sp", K["XH"][:, :], hlf[:, :], w=[P.dk("XH")], r=[hlf.tk])
    P.run()


def phase_ht_from_r(G, K):
    P = Phase(G, "ht")
    T = LNTail.__new__(LNTail)
    T.P, T.K, T.final, T.router = P, K, False, None
    T.ident = P.sb([128, 128], F32)
    P.dma("sp", T.ident[:, :], K["ident"][:, :], w=[T.ident.tk])
    T.pst = [P.ps([128, 1024], F32)]
    T.hto = [P.sb([128, 8, 512], BF16) for _ in range(2)]
    T.htcnt = 0
    T.cnt = 0
    xb = [P.sb([128, D], F32) for _ in range(3)]
    for si, sub in enumerate(subtiles()):
        row0, rows, httile, col0 = sub
        b = xb[si % 3]
        P.dma("sp", b[:rows, :], K["R"][row0:row0 + rows, :], w=[b.tk])
        T.transpose(b, b.tk, sub)
    P.run()


RG_TT = 256
RG_NT = SEG // RG_TT


def rg_tiles():
    out = [(RG_NT, NMETA, 8, 0)]
    for n in range(RG_NT):
        out.append((n, RG_TT, n // 2, (n % 2) * RG_TT))
    return out


def phase_rg1(G, K, j):
    P = Phase(G, "rg1")
    nc = P.nc
    win = P.sb([128, 8, RGW], BF16)
    P.dma("pool", win[:, :, :], K["rg_w_in"][j].rearrange("(kc p) c -> p kc c", p=128)[:, :, RGW:2 * RGW], w=[win.tk])
    wg = P.sb([CT, 2 * 8 * 2 * 168], BF16)
    P.dma("pool", wg[:, :].rearrange("p (a b) -> p a b", b=1344), K["rg_wg"][j].rearrange("p (a b) -> p a b", b=1344), w=[wg.tk])
    prm = P.sb([CT, 8 * NCT], F32)
    P.dma("sp", prm[:, :], K["rg_prm"][j], w=[prm.tk])
    selw = P.sb([128, 32], F32)
    P.dma("sp", selw[:, :], K["selw"][:, :], w=[selw.tk])

    def pr(k, t):
        return prm[:, k * NCT + t:k * NCT + t + 1]
    cch = P.sb([CT, NCT], F32)
    cch2 = P.sb([CT, NCT], F32)
    P.act(lambda e: e.activation(out=cch[:, :], in_=prm[:, 7 * NCT:8 * NCT], func=AF.Exp, scale=-1.0), w=[cch.tk], r=[prm.tk])
    P.act(lambda e: e.activation(out=cch[:, :], in_=cch[:, :], func=AF.Ln, bias=1.0), w=[cch.tk], r=[cch.tk])
    P.dve(lambda e: e.tensor_scalar(out=cch2[:, :], in0=cch[:, :], scalar1=-16.0, scalar2=None, op0=ALU.mult),
          w=[cch2.tk], r=[cch.tk])
    P.dve(lambda e: e.tensor_scalar(out=cch[:, :], in0=cch[:, :], scalar1=-8.0, scalar2=None, op0=ALU.mult),
          w=[cch.tk], r=[cch.tk])

    hlm = P.sb([128, 8, 3], BF16)
    P.dma("sp", hlm[:, :, :], K["HT"][8, :, :].rearrange("p (j c) -> p j c", j=8)[:, :, 13:16], w=[hlm.tk], r=[P.dk("HT", 8)])
    gh = xch_load(P, K, "XHg", 128, 24)
    hsel = P.sb([128, 24], F32)
    P.dve(lambda e: e.tensor_copy(out=hsel[:, :].rearrange("p (j c) -> p j c", j=8), in_=hlm[:, :, :]), w=[hsel.tk], r=[hlm.tk])
    P.dve(lambda e: e.tensor_scalar(out=hsel[:, :], in0=hsel[:, :], scalar1=selw[:, 16:17], scalar2=None, op0=ALU.mult),
          w=[hsel.tk], r=[hsel.tk, selw.tk])
    for r in range(NCORE):
        P.dve(lambda e, r=r: e.scalar_tensor_tensor(out=hsel[:, :], in0=gh[:, r, :], scalar=selw[:, 8 + r:9 + r], in1=hsel[:, :],
                                                    op0=ALU.mult, op1=ALU.add), w=[hsel.tk], r=[hsel.tk, gh.tk, selw.tk])
    hsb = P.sb([128, 8, 3], BF16)
    P.dve(lambda e: e.tensor_copy(out=hsb[:, :, :], in_=hsel[:, :].rearrange("p (j c) -> p j c", j=8)), w=[hsb.tk], r=[hsel.tk])

    W = RG_TT
    XR = [P.sb([CT, NCT, W + 3], F32) for _ in range(2)]
    XC = P.sb([CT, NCT, W], F32)
    XCb = P.sb([CT, NCT, W], BF16, ntk=8)
    Rg = P.sb([CT, NCT, W], F32)
    Ig = P.sb([CT, NCT, W], F32)
    A = [P.sb([CT, NCT, W], F32)] * 2
    S = P.sb([CT, NCT, W], F32)
    U = [P.sb([CT, NCT, W], F32)] * 2
    HS = S
    hT = [P.sb([128, 8, W], BF16) for _ in range(2)]
    state = P.sb([CT, NCT], F32)
    rstot = P.sb([CT, NCT], F32)
    rsum = P.sb([CT, NCT], F32)
    hmeta = P.sb([CT, NCT], F32)
    pss = [P.ps([128, 512], F32) for _ in range(8)]
    pcnt = [0]

    def nps():
        p = pss[pcnt[0] % 8]
        pcnt[0] += 1
        return p

    P.dve(lambda e: e.memset(state[:, :], 0.0), w=[state.tk])
    P.dve(lambda e: e.memset(rstot[:, :], 0.0), w=[rstot.tk])

    def tile_body(ti, n, w, httile, hc0):
        xr = XR[ti % 2]
        xrp = XR[(ti - 1) % 2]
        a = A[ti % 2]
        u = U[ti % 2]
        h = hT[ti % 2]
        P.dma("sp", h[:, :, 0:w], K["HT"][httile, :, :].rearrange("p (j c) -> p j c", j=8)[:, :, hc0:hc0 + w],
              w=[h.tk], r=[P.dk("HT", httile)])
        if ti == 0:
            P.dve(lambda e, xr=xr: e.memset(xr[:, :, 0:3], 0.0), w=[xr.tk])
        elif ti == 1:
            for t in range(NCT):
                p = nps()
                for kc in range(8):
                    P.pe(lambda e, t=t, kc=kc, p=p: e.matmul(p[:CT, 0:3], lhsT=win[:, kc, CT * t:CT * t + CT], rhs=hsb[:, kc, :],
                                                             start=(kc == 0), stop=(kc == 7)), w=[p.tk], r=[win.tk, hsb.tk])
                P.act(lambda e, t=t, p=p, xr=xr: e.copy(out=xr[:, t, 0:3], in_=p[:CT, 0:3]), w=[xr.tk], r=[p.tk])
        else:
            P.dve(lambda e, xr=xr, xrp=xrp: e.tensor_copy(out=xr[:, :, 0:3], in_=xrp[:, :, W:W + 3]), w=[xr.tk], r=[xrp.tk])
        if ti == 1 and "dbg" in K and j == 0:
            P.dma("sp", K["dbg"][0:CT, 32:80].rearrange("p (t c) -> p t c", t=NCT), xr[:, :, 0:3], w=[P.dk("dbg", 1)], r=[xr.tk])
            P.dma("sp", K["dbg"][:, 128:152], hsel[:, :], w=[P.dk("dbg", 2)], r=[hsel.tk])
        for t in range(NCT):
            p = nps()
            for kc in range(8):
                P.pe(lambda e, t=t, kc=kc, p=p, h=h: e.matmul(p[:CT, 0:w], lhsT=win[:, kc, CT * t:CT * t + CT], rhs=h[:, kc, 0:w],
                                                             start=(kc == 0), stop=(kc == 7)), w=[p.tk], r=[win.tk, h.tk])
            P.act(lambda e, t=t, p=p, xr=xr: e.copy(out=xr[:, t, 3:3 + w], in_=p[:CT, 0:w]), w=[xr.tk], r=[p.tk])
        for t in range(NCT):
            P.dve(lambda e, t=t, xr=xr: e.tensor_scalar(out=XC[:, t, 0:w], in0=xr[:, t, 0:w], scalar1=pr(0, t), scalar2=pr(4, t),
                                                        op0=ALU.mult, op1=ALU.add), w=[XC.tk], r=[xr.tk, prm.tk])
            for k in range(1, 4):
                P.dve(lambda e, t=t, k=k, xr=xr: e.scalar_tensor_tensor(out=XC[:, t, 0:w], in0=xr[:, t, k:k + w], scalar=pr(k, t),
                                                                        in1=XC[:, t, 0:w], op0=ALU.mult, op1=ALU.add),
                      w=[XC.tk], r=[xr.tk, XC.tk, prm.tk])
        for b in range(8):
            P.act(lambda e, b=b: e.copy(out=XCb[:, 2 * b:2 * b + 2, 0:w], in_=XC[:, 2 * b:2 * b + 2, 0:w]), w=[XCb.tks[b]], r=[XC.tk])
        for g, (dst, kb) in enumerate(((Rg, 5), (Ig, 6))):
            for t in range(NCT):
                n8 = t // 2
                dl = (t % 2) * CT
                p = nps()
                for ci in range(2):
                    off = ((g * 8 + n8) * 2 + ci) * 168 + dl
                    P.pe(lambda e, off=off, ci=ci, n8=n8, p=p: e.matmul(p[:CT, 0:w], lhsT=wg[:, off:off + CT], rhs=XCb[:, 2 * n8 + ci, 0:w],
                                                                       start=(ci == 0), stop=(ci == 1)), w=[p.tk], r=[wg.tk, XCb.tks[n8]])
                P.act(lambda e, t=t, p=p, dst=dst, kb=kb: e.activation(out=dst[:, t, 0:w], in_=p[:CT, 0:w], func=AF.Sigmoid, bias=pr(kb, t)),
                      w=[dst.tk], r=[p.tk, prm.tk])
        for t in range(NCT):
            P.act(lambda e, t=t, a=a: e.activation(out=a[:, t, 0:w], in_=Rg[:, t, 0:w], func=AF.Exp, scale=cch[:, t:t + 1]),
                  w=[a.tk], r=[Rg.tk, cch.tk])
        for t in range(NCT):
            P.act(lambda e, t=t: e.activation(out=S[:, t, 0:w], in_=Rg[:, t, 0:w], func=AF.Tanh, scale=cch[:, t:t + 1]),
                  w=[S.tk], r=[Rg.tk, cch.tk])
        P.dve(lambda e, a=a, u=u: e.tensor_tensor(out=u[:, :, 0:w], in0=a[:, :, 0:w], in1=a[:, :, 0:w], op=ALU.mult), w=[u.tk], r=[a.tk])
        P.dve(lambda e, u=u: e.scalar_tensor_tensor(out=S[:, :, 0:w], in0=u[:, :, 0:w], scalar=1.0, in1=S[:, :, 0:w], op0=ALU.add, op1=ALU.mult),
              w=[S.tk], r=[u.tk, S.tk])
        P.act(lambda e: e.activation(out=S[:, :, 0:w], in_=S[:, :, 0:w], func=AF.Sqrt, scale=-1.0), w=[S.tk], r=[S.tk])
        P.dve(lambda e, u=u: e.tensor_tensor(out=u[:, :, 0:w], in0=S[:, :, 0:w], in1=Ig[:, :, 0:w], op=ALU.mult), w=[u.tk], r=[S.tk, Ig.tk])
        P.dve(lambda e, u=u: e.tensor_tensor(out=u[:, :, 0:w], in0=u[:, :, 0:w], in1=XC[:, :, 0:w], op=ALU.mult), w=[u.tk], r=[u.tk, XC.tk])
        P.dve(lambda e: e.reduce_sum(out=rsum[:, :], in_=Rg[:, :, 0:w], axis=AX.X), w=[rsum.tk], r=[Rg.tk])
        P.dve(lambda e: e.tensor_tensor(out=rstot[:, :], in0=rstot[:, :], in1=rsum[:, :], op=ALU.add), w=[rstot.tk], r=[rstot.tk, rsum.tk])
        for t in range(NCT):
            P.dve(lambda e, t=t, a=a, u=u: e.tensor_tensor_scan(out=HS[:, t, 0:w], data0=a[:, t, 0:w], data1=u[:, t, 0:w],
                                                                initial=state[:, t:t + 1], op0=ALU.mult, op1=ALU.add),
                  w=[HS.tk], r=[a.tk, u.tk, state.tk])
        P.dve(lambda e: e.tensor_copy(out=state[:, :], in_=HS[:, :, w - 1]), w=[state.tk], r=[HS.tk])
        P.dma("sp", K["AU"][0, n, :, :].rearrange("p (t c) -> p t c", t=NCT)[:, :, 0:w], a[:, :, 0:w], w=[P.dk("AU", 0, n)], r=[a.tk])
        P.dma("sp", K["AU"][1, n, :, :].rearrange("p (t c) -> p t c", t=NCT)[:, :, 0:w], u[:, :, 0:w], w=[P.dk("AU", 1, n)], r=[u.tk])
        if ti == 0:
            P.dve(lambda e: e.tensor_copy(out=hmeta[:, :], in_=state[:, :]), w=[hmeta.tk], r=[state.tk])
            if "dbg" in K and j == 0:
                P.dma("sp", K["dbg"][0:CT, 0:NCT], hmeta[:, :], w=[P.dk("dbg", 0)], r=[hmeta.tk])
                P.dma("sp", K["dbg"][0:CT, 256:275], xr[:, 0, 0:19], w=[P.dk("dbg", 10)], r=[xr.tk])
                P.dma("sp", K["dbg"][0:CT, 288:304], XC[:, 0, 0:16], w=[P.dk("dbg", 11)], r=[XC.tk])
                P.dma("sp", K["dbg"][0:CT, 304:320], Rg[:, 0, 0:16], w=[P.dk("dbg", 12)], r=[Rg.tk])
                P.dma("sp", K["dbg"][0:CT, 320:336], Ig[:, 0, 0:16], w=[P.dk("dbg", 13)], r=[Ig.tk])
                P.dma("sp", K["dbg"][0:CT, 336:352], a[:, 0, 0:16], w=[P.dk("dbg", 14)], r=[a.tk])
                P.dma("sp", K["dbg"][0:CT, 352:368], u[:, 0, 0:16], w=[P.dk("dbg", 15)], r=[u.tk])
                P.dma("sp", K["dbg"][0:CT, 368:384], HS[:, 0, 0:16], w=[P.dk("dbg", 16)], r=[HS.tk])
                P.dma("sp", K["dbg"][0:CT, 384:400], cch[:, :], w=[P.dk("dbg", 17)], r=[cch.tk])
            P.dve(lambda e: e.memset(state[:, :], 0.0), w=[state.tk])
            P.dve(lambda e: e.memset(rstot[:, :], 0.0), w=[rstot.tk])

    for ti, (n, w, httile, hc0) in enumerate(rg_tiles()):
        tile_body(ti, n, w, httile, hc0)

    summ = P.sb([CT, 2 * NCT], F32)
    P.dve(lambda e: e.tensor_tensor(out=summ[:, 0:NCT], in0=rstot[:, :], in1=cch[:, :], op=ALU.mult), w=[summ.tk], r=[rstot.tk, cch.tk])
    P.act(lambda e: e.activation(out=summ[:, 0:NCT], in_=summ[:, 0:NCT], func=AF.Exp), w=[summ.tk], r=[summ.tk])
    P.dve(lambda e: e.tensor_copy(out=summ[:, NCT:2 * NCT], in_=state[:, :]), w=[summ.tk], r=[state.tk, summ.tk])
    P.dma("sp", K["XS"][:, :], summ[:, :], w=[P.dk("XS")], r=[summ.tk])
    P.dma("sp", K["HMETA"][:, :], hmeta[:, :], w=[P.dk("HMETA")], r=[hmeta.tk])
    P.run()


def phase_rg2(G, K, j, li):
    P = Phase(G, "rg2")
    T = LNTail(P, K, li, 0)
    win = P.sb([128, 8, RGW], BF16)
    P.dma("pool", win[:, :, :], K["rg_w_in"][j].rearrange("(kc p) c -> p kc c", p=128)[:, :, 0:RGW], w=[win.tk])
    wout = P.sb([CT, NCT, D], BF16)
    P.dma("pool", wout[:, :, :], K["rg_w_out"][j].rearrange("(t p) d -> p t d", p=CT), w=[wout.tk])
    W = RG_TT
    A = [P.sb([CT, NCT, W], F32) for _ in range(2)]
    U = [P.sb([CT, NCT, W], F32) for _ in range(2)]
    HS = P.sb([CT, NCT, W], F32)
    GG = P.sb([CT, NCT, W], BF16)
    Y = P.sb([CT, NCT, W], BF16)
    hT = [P.sb([128, 8, W], BF16) for _ in range(2)]
    Rr = [P.sb([128, D], F32) for _ in range(2)]
    acc = [P.sb([128, D], F32) for _ in range(2)]
    state = P.sb([CT, NCT], F32)
    hin = P.sb([CT, NCT], F32)
    P.dma("sp", hin[:, :], K["HMETA"][:, :], w=[hin.tk], r=[P.dk("HMETA")])
    selw = P.sb([128, 32], F32)
    P.dma("sp", selw[:, :], K["selw"][:, :], w=[selw.tk])
    gs = xch_load(P, K, "XSg", CT, 2 * NCT)
    t1 = P.sb([CT, NCT], F32)
    for r in range(NCORE):
        P.dve(lambda e, r=r: e.tensor_scalar(out=t1[:, :], in0=gs[:, r, 0:NCT], scalar1=1.0, scalar2=selw[:CT, r:r + 1],
                                             op0=ALU.subtract, op1=ALU.mult), w=[t1.tk], r=[gs.tk, selw.tk])
        P.dve(lambda e: e.scalar_tensor_tensor(out=hin[:, :], in0=t1[:, :], scalar=1.0, in1=hin[:, :], op0=ALU.add, op1=ALU.mult),
              w=[hin.tk], r=[t1.tk, hin.tk])
        P.dve(lambda e, r=r: e.scalar_tensor_tensor(out=hin[:, :], in0=gs[:, r, NCT:2 * NCT], scalar=selw[:CT, r:r + 1], in1=hin[:, :],
                                                    op0=ALU.mult, op1=ALU.add), w=[hin.tk], r=[gs.tk, hin.tk, selw.tk])
    pss = [P.ps([128, 512], F32) for _ in range(6)]
    pcnt = [0]

    def nps():
        p = pss[pcnt[0] % 6]
        pcnt[0] += 1
        return p
    P.dve(lambda e: e.memset(state[:, :], 0.0), w=[state.tk])
    subs = subtiles()
    scnt = 0

    def tile_body(ti, n, w, httile, hc0):
        nonlocal scnt
        a, u, h = A[ti % 2], U[ti % 2], hT[ti % 2]
        P.dma("sp", h[:, :, 0:w], K["HT"][httile, :, :].rearrange("p (j c) -> p j c", j=8)[:, :, hc0:hc0 + w],
              w=[h.tk], r=[P.dk("HT", httile)])
        P.dma("sp", a[:, :, 0:w], K["AU"][0, n, :, :].rearrange("p (t c) -> p t c", t=NCT)[:, :, 0:w], w=[a.tk], r=[P.dk("AU", 0, n)])
        P.dma("sp", u[:, :, 0:w], K["AU"][1, n, :, :].rearrange("p (t c) -> p t c", t=NCT)[:, :, 0:w], w=[u.tk], r=[P.dk("AU", 1, n)])
        if ti == 1:
            P.dve(lambda e: e.tensor_copy(out=state[:, :], in_=hin[:, :]), w=[state.tk], r=[hin.tk])
        for t in range(NCT):
            P.dve(lambda e, t=t, a=a, u=u: e.tensor_tensor_scan(out=HS[:, t, 0:w], data0=a[:, t, 0:w], data1=u[:, t, 0:w],
                                                                initial=state[:, t:t + 1], op0=ALU.mult, op1=ALU.add),
                  w=[HS.tk], r=[a.tk, u.tk, state.tk])
        P.dve(lambda e: e.tensor_copy(out=state[:, :], in_=HS[:, :, w - 1]), w=[state.tk], r=[HS.tk])
        for t in range(NCT):
            p = nps()
            for kc in range(8):
                P.pe(lambda e, t=t, kc=kc, p=p, h=h: e.matmul(p[:CT, 0:w], lhsT=win[:, kc, CT * t:CT * t + CT], rhs=h[:, kc, 0:w],
                                                             start=(kc == 0), stop=(kc == 7)), w=[p.tk], r=[win.tk, h.tk])
            P.act(lambda e, t=t, p=p: e.activation(out=GG[:, t, 0:w], in_=p[:CT, 0:w], func=AF.Gelu_apprx_tanh), w=[GG.tk], r=[p.tk])
        P.dve(lambda e: e.tensor_tensor(out=Y[:, :, 0:w], in0=HS[:, :, 0:w], in1=GG[:, :, 0:w], op=ALU.mult), w=[Y.tk], r=[HS.tk, GG.tk])
        nsub = (w + 127) // 128
        for s in range(nsub):
            if w == NMETA:
                sub = subs[32]
            else:
                sub = subs[n * (RG_TT // 128) + s]
            row0, rows, _, _ = sub
            rr, ac = Rr[scnt % 2], acc[scnt % 2]
            scnt += 1
            P.dma("sp", rr[:rows, :], K["R"][row0:row0 + rows, :], w=[rr.tk], r=[P.dk("R", row0)])
            for hf in range(2):
                p = nps()
                for t in range(NCT):
                    P.pe(lambda e, t=t, hf=hf, p=p, s=s, rows=rows: e.matmul(p[:rows, :], lhsT=Y[:, t, s * 128:s * 128 + rows],
                                                                            rhs=wout[:, t, hf * 512:(hf + 1) * 512],
                                                                            start=(t == 0), stop=(t == NCT - 1)), w=[p.tk], r=[Y.tk, wout.tk])
                P.dve(lambda e, hf=hf, p=p, rr=rr, ac=ac, rows=rows: e.scalar_tensor_tensor(
                    out=ac[:rows, hf * 512:(hf + 1) * 512], in0=rr[:rows, hf * 512:(hf + 1) * 512], scalar=ALPHA,
                    in1=p[:rows, :], op0=ALU.mult, op1=ALU.add), w=[ac.tk], r=[rr.tk, p.tk])
            T.run(ac[:rows, :], ac.tk, sub)
    for ti, (n, w, httile, hc0) in enumerate(rg_tiles()):
        tile_body(ti, n, w, httile, hc0)
    P.run()


def phase_ffn(G, K, j, li, moe, final=False):
    P = Phase(G, "ffn")
    T = LNTail(P, K, li, 1, final=final)
    E = NEXP if moe else 1
    subs = subtiles()
    hT = [P.sb([128, 8, 512], BF16) for _ in range(3)]
    acc = P.sb([128, 9, D], F32, ntk=9)
    wgb = [P.sb([128, 8, 512], BF16) for _ in range(2)]
    wub = [P.sb([128, 8, 512], BF16) for _ in range(2)]
    wob = [P.sb([128, 4, D], BF16) for _ in range(2)]
    actb = [P.sb([128, 4, 1040], BF16) for _ in range(2)]
    sg = [P.sb([128, 512], F32) for _ in range(2)]
    psg = [P.ps([128, 512], F32) for _ in range(2)]
    psu = [P.ps([128, 512], F32) for _ in range(2)]
    pso = [P.ps([128, 512], F32) for _ in range(2)]
    if moe:
        gt = P.sb([128, 33, NEXP], F32)
        P.dma("sp", gt[:, 0:32, :], K["GT"][0:SEG, :].rearrange("(s p) e -> p s e", p=128), w=[gt.tk], r=[P.dk("GT", "all")])
        P.dma("sp", gt[:NMETA, 32, :], K["GT"][SEG:NTOK, :], w=[gt.tk], r=[P.dk("GT", "all")])
    if moe:
        win_all, wout_all = K["moe_w_in"][j], K["moe_w_out"][j]
    else:
        win_all, wout_all = K["ffn_w_in"][j:j + 1], K["ffn_w_out"][j:j + 1]

    iters = [(g, e, fg) for g in range(4) for e in range(E) for fg in range(7)]

    def load_w(it):
        _, e, fg = iters[it]
        s = it % 2
        wi = win_all[e].rearrange("(kc p) c -> p kc c", p=128)
        P.dma("pool", wgb[s][:, :, :], wi[:, :, fg * 512:(fg + 1) * 512], w=[wgb[s].tk])
        P.dma("pool", wub[s][:, :, :], wi[:, :, DFF + fg * 512:DFF + (fg + 1) * 512], w=[wub[s].tk])
        P.dma("pool", wob[s][:, :, :], wout_all[e][fg * 512:(fg + 1) * 512, :].rearrange("(fi p) d -> p fi d", p=128), w=[wob[s].tk])

    load_w(0)
    cnt = 0
    ocnt = 0
    for it, (g, e, fg) in enumerate(iters):
        gsubs = list(range(8 * g, 8 * g + 8)) + ([32] if g == 3 else [])
        mts = [(0, 512, 0), (1, 512, 512)] + ([(2, NMETA, 1024)] if g == 3 else [])
        if e == 0 and fg == 0:
            P.dma("sp", hT[0][:, :, :], K["HT"][2 * g, :, :].rearrange("p (j c) -> p j c", j=8), w=[hT[0].tk], r=[P.dk("HT", 2 * g)])
            P.dma("sp", hT[1][:, :, :], K["HT"][2 * g + 1, :, :].rearrange("p (j c) -> p j c", j=8), w=[hT[1].tk], r=[P.dk("HT", 2 * g + 1)])
            if g == 3:
                P.dma("sp", hT[2][:, :, 0:NMETA], K["HT"][8, :, :].rearrange("p (j c) -> p j c", j=8)[:, :, 0:NMETA],
                      w=[hT[2].tk], r=[P.dk("HT", 8)])
            for k, si in enumerate(gsubs):
                row0, rows, _, _ = subs[si]
                P.dma("sp", acc[:rows, k, :], K["R"][row0:row0 + rows, :], w=[acc.tks[k]], r=[P.dk("R", row0)])
                P.act(lambda e_, k=k, rows=rows: e_.mul(out=acc[:rows, k, :], in_=acc[:rows, k, :], mul=ALPHA), w=[acc.tks[k]], r=[acc.tks[k]])
        if it + 1 < len(iters):
            load_w(it + 1)
        s = it % 2
        ab = actb[s]
        for fi in range(4):
            for (hb, w, c0) in mts:
                pg, pu, sgb = psg[cnt % 2], psu[cnt % 2], sg[cnt % 2]
                cnt += 1
                for kc in range(8):
                    P.pe(lambda e_, fi=fi, kc=kc, pg=pg, hb=hb, w=w, s=s: e_.matmul(pg[:, 0:w], lhsT=wgb[s][:, kc, fi * 128:(fi + 1) * 128],
                                                                                 rhs=hT[hb][:, kc, 0:w], start=(kc == 0), stop=(kc == 7)),
                         w=[pg.tk], r=[wgb[s].tk, hT[hb].tk])
                for kc in range(8):
                    P.pe(lambda e_, fi=fi, kc=kc, pu=pu, hb=hb, w=w, s=s: e_.matmul(pu[:, 0:w], lhsT=wub[s][:, kc, fi * 128:(fi + 1) * 128],
                                                                                 rhs=hT[hb][:, kc, 0:w], start=(kc == 0), stop=(kc == 7)),
                         w=[pu.tk], r=[wub[s].tk, hT[hb].tk])
                P.act(lambda e_, pg=pg, sgb=sgb, w=w: e_.activation(out=sgb[:, 0:w], in_=pg[:, 0:w], func=AF.Silu), w=[sgb.tk], r=[pg.tk])
                P.dve(lambda e_, pu=pu, sgb=sgb, w=w, fi=fi, c0=c0, ab=ab: e_.tensor_tensor(out=ab[:, fi, c0:c0 + w], in0=sgb[:, 0:w],
                                                                                         in1=pu[:, 0:w], op=ALU.mult),
                      w=[ab.tk], r=[sgb.tk, pu.tk])
        for k, si in enumerate(gsubs):
            row0, rows, _, _ = subs[si]
            c0 = k * 128
            for hf in range(2):
                po = pso[ocnt % 2]
                ocnt += 1
                for fi in range(4):
                    P.pe(lambda e_, fi=fi, po=po, c0=c0, rows=rows, hf=hf, s=s, ab=ab: e_.matmul(
                        po[:rows, :], lhsT=ab[:, fi, c0:c0 + rows], rhs=wob[s][:, fi, hf * 512:(hf + 1) * 512],
                        start=(fi == 0), stop=(fi == 3)), w=[po.tk], r=[ab.tk, wob[s].tk])
                sc = gt[:rows, si, e:e + 1] if moe else 1.0
                rd = [po.tk, acc.tks[k]] + ([gt.tk] if moe else [])
                P.dve(lambda e_, po=po, rows=rows, hf=hf, k=k, sc=sc: e_.scalar_tensor_tensor(
                    out=acc[:rows, k, hf * 512:(hf + 1) * 512], in0=po[:rows, :], scalar=sc, in1=acc[:rows, k, hf * 512:(hf + 1) * 512],
                    op0=ALU.mult, op1=ALU.add), w=[acc.tks[k]], r=rd)
        if e == E - 1 and fg == 6:
            for k, si in enumerate(gsubs):
                rows = subs[si][1]
                T.run(acc[:rows, k, :], acc.tks[k], subs[si])
    P.run()


EXT_SHAPES = {
    "x": [SEG, D], "meta": [NMETA, D], "ident": [128, 128], "selw": [128, 32], "cmask": [128, 128],
    "ln_gain": [DEPTH * 2, D], "ln_bias": [DEPTH * 2, D],
    "rg_w_in": [2, D, 2 * RGW], "rg_wg": [2, CT, 2 * 8 * 2 * 168], "rg_prm": [2, CT, 8 * NCT], "rg_w_out": [2, RGW, D],
    "gla_w_in": [2, D, GIN], "gla_w_gate_up": [2, 16, GQK], "gla_b_gate": [2, 128, 4], "gla_norm_g": [2, GVD], "gla_w_out": [2, GVD, D],
    "ffn_w_in": [2, D, 2 * DFF], "ffn_w_out": [2, DFF, D],
    "moe_router": [2, D, NEXP], "moe_w_in": [2, NEXP, D, 2 * DFF], "moe_w_out": [2, NEXP, DFF, D],
}


class KD(dict):
    def __init__(self, nc):
        super().__init__()
        self.nc = nc

    def __missing__(self, name):
        ap = self.nc.dram_tensor(name, list(EXT_SHAPES[name]), F32, kind="ExternalInput").ap()
        self[name] = ap
        return ap


def gla_chunks():
    out = [(32, NMETA)]
    for c in range(32):
        out.append((c, 128))
    return out


def phase_gla(G, K, j, li, pss):
    P = Phase(G, "gla%d" % pss)
    full = pss == 2
    subs = subtiles()
    if full:
        T = LNTail(P, K, li, 0, router=K["moe_router"][j])
        ident = T.ident
    else:
        ident = P.sb([128, 128], F32)
        P.dma("sp", ident[:, :], K["ident"][:, :], w=[ident.tk])
    win = P.sb([128, 8, 3200], BF16)
    P.dve(lambda e: e.memset(win[:, :, 3072:3200], 0.0), w=[win.tk])
    wv = K["gla_w_in"][j].rearrange("(kc p) c -> p kc c", p=128)
    for (c0, c1) in ((0, 1024), (1024, 2048), (2048, GIN)):
        if not full and c0 == 0:
            P.dma("pool", win[:, :, 512:1024], wv[:, :, 512:1024], w=[win.tk])
        elif not full and c0 == 2048:
            P.dma("pool", win[:, :, 3072:GIN], wv[:, :, 3072:GIN], w=[win.tk])
        else:
            P.dma("pool", win[:, :, c0:c1], wv[:, :, c0:c1], w=[win.tk])
    wup = P.sb([128, GQK], BF16)
    P.dve(lambda e: e.memset(wup[:, :], 0.0), w=[wup.tk])
    P.dma("pool", wup[0:16, :], K["gla_w_gate_up"][j], w=[wup.tk])
    negb = P.sb([128, 4], F32)
    P.dma("sp", negb[:, :], K["gla_b_gate"][j], w=[negb.tk])
    P.dve(lambda e: e.tensor_scalar(out=negb[:, :], in0=negb[:, :], scalar1=-1.0, scalar2=None, op0=ALU.mult), w=[negb.tk], r=[negb.tk])
    ones = P.sb([128, 128], F32)
    P.dve(lambda e: e.memset(ones[:, :], 1.0), w=[ones.tk])
    if full:
        wout = P.sb([128, 8, D], BF16)
        P.dma("pool", wout[:, :, :], K["gla_w_out"][j].rearrange("(kc p) d -> p kc d", p=128), w=[wout.tk])
        ng = P.sb([128, GVD], F32)
        P.dma("sp", ng[:, :], K["gla_norm_g"][j, :].partition_broadcast(128), w=[ng.tk])
        cm4 = P.sb([128, 512], F32)
        for h in range(4):
            P.dma("sp", cm4[:, h * 128:(h + 1) * 128], K["cmask"][:, :], w=[cm4.tk])
    hT = [P.sb([128, 8, 128], BF16) for _ in range(2)]
    zT = P.sb([128, 128], BF16)
    lg = P.sb([128, 4, 128], F32)
    cs = P.sb([128, 4, 128], F32)
    sm = P.sb([128, 16], F32)
    Eko = P.sb([128, 4, 128], F32)
    ko = P.sb([128, 4, 128], F32)
    koT = P.sb([128, 512], BF16)
    vbf = P.sb([128, GVD], BF16)
    S = P.sb([128, 4, 256], F32, ntk=4)
    cstot = P.sb([128, 4], F32)
    if full:
        Eqi = P.sb([128, 4, 128], F32)
        Eki = P.sb([128, 4, 128], F32)
        Eqn = P.sb([128, 4, 128], F32)
        qt = P.sb([128, 4, 128], BF16)
        kt = P.sb([128, 4, 128], BF16)
        qn = P.sb([128, 4, 128], BF16)
        rs = P.sb([128, GVD], F32)
        scb = P.sb([128, 512], BF16)
        Sbf = P.sb([128, 4, 256], BF16, ntk=4)
        og = [P.sb([128, GVD], F32) for _ in range(2)]
        ogT = P.sb([128, 8, 128], BF16)
        st6 = P.sb([128, 4, 6], F32)
        mv = P.sb([128, 4, 2], F32)
        rstd = P.sb([128, 4], F32)
        Rr = [P.sb([128, D], F32) for _ in range(2)]
        acc = [P.sb([128, D], F32) for _ in range(2)]
    npsb = 5 if full else 8
    pss_ = [P.ps([128, 512], F32) for _ in range(npsb)]
    pcnt = [0]

    def nps():
        p = pss_[pcnt[0] % npsb]
        pcnt[0] += 1
        return p

    for h in range(4):
        P.dve(lambda e, h=h: e.memset(S[:, h, :], 0.0), w=[S.tks[h]])
        if full:
            P.dve(lambda e, h=h: e.memset(Sbf[:, h, :], 0.0), w=[Sbf.tks[h]])
    P.dve(lambda e: e.memset(cstot[:, :], 0.0), w=[cstot.tk])

    def inproj_fm(hb, w, c0, m, p, col0):
        for kc in range(8):
            P.pe(lambda e, kc=kc: e.matmul(p[:m, col0:col0 + w], lhsT=win[:, kc, c0:c0 + m], rhs=hb[:, kc, 0:w],
                                           start=(kc == 0), stop=(kc == 7)), w=[p.tk], r=[win.tk, hb.tk])

    def chunk_body(ci, c, w):
        hb = hT[ci % 2]
        sub = subs[c]
        row0 = sub[0]
        httile, hc0 = sub[2], sub[3]
        mid = w // 2 - 1
        P.dma("sp", hb[:, :, 0:w], K["HT"][httile, :, :].rearrange("p (j c) -> p j c", j=8)[:, :, hc0:hc0 + w],
              w=[hb.tk], r=[P.dk("HT", httile)])
        if full:
            pq = nps()
            for h in range(4):
                inproj_fm(hb, w, h * 128, 128, pq, h * 128)
        pk = nps()
        for h in range(4):
            inproj_fm(hb, w, GQK + h * 128, 128, pk, h * 128)
        pz = nps()
        inproj_fm(hb, w, 3072, 128, pz, 0)
        P.act(lambda e: e.copy(out=zT[:, 0:w], in_=pz[:, 0:w]), w=[zT.tk], r=[pz.tk])
        plg = nps()
        for h in range(4):
            P.pe(lambda e, h=h: e.matmul(plg[:, h * 128:h * 128 + w], lhsT=wup[:, h * 128:(h + 1) * 128], rhs=zT[:, 0:w],
                                         start=True, stop=True), w=[plg.tk], r=[wup.tk, zT.tk])
        for h in range(4):
            P.act(lambda e, h=h: e.activation(out=lg[:, h, 0:w], in_=plg[:, h * 128:h * 128 + w], func=AF.Exp, scale=-1.0,
                                              bias=negb[:, h:h + 1]), w=[lg.tk], r=[plg.tk, negb.tk])
        P.act(lambda e: e.activation(out=lg[:, :, 0:w], in_=lg[:, :, 0:w], func=AF.Ln, bias=1.0), w=[lg.tk], r=[lg.tk])
        for h in range(4):
            P.dve(lambda e, h=h: e.tensor_tensor_scan(out=cs[:, h, 0:w], data0=ones[:, 0:w], data1=lg[:, h, 0:w], initial=0.0,
                                                      op0=ALU.mult, op1=ALU.add), w=[cs.tk], r=[ones.tk, lg.tk])
        P.dve(lambda e: e.tensor_scalar(out=sm[:, 0:4], in0=cs[:, :, mid], scalar1=1.0 / 16, scalar2=None, op0=ALU.mult), w=[sm.tk], r=[cs.tk])
        P.dve(lambda e: e.tensor_scalar(out=sm[:, 4:8], in0=cs[:, :, mid], scalar1=-1.0 / 16, scalar2=None, op0=ALU.mult), w=[sm.tk], r=[cs.tk])
        P.dve(lambda e: e.tensor_scalar(out=sm[:, 8:12], in0=cs[:, :, w - 1], scalar1=-1.0 / 16, scalar2=None, op0=ALU.mult), w=[sm.tk], r=[cs.tk])
        P.dve(lambda e: e.tensor_tensor(out=cstot[:, :], in0=cstot[:, :], in1=cs[:, :, w - 1], op=ALU.add), w=[cstot.tk], r=[cstot.tk, cs.tk])
        P.act(lambda e: e.activation(out=sm[:, 12:16], in_=sm[:, 8:12], func=AF.Exp), w=[sm.tk], r=[sm.tk])
        for h in range(4):
            P.act(lambda e, h=h: e.activation(out=Eko[:, h, 0:w], in_=cs[:, h, 0:w], func=AF.Exp, scale=1.0 / 16, bias=sm[:, 8 + h:9 + h]),
                  w=[Eko.tk], r=[cs.tk, sm.tk])
            if full:
                P.act(lambda e, h=h: e.activation(out=Eqi[:, h, 0:w], in_=cs[:, h, 0:w], func=AF.Exp, scale=-1.0 / 16, bias=sm[:, h:h + 1]),
                      w=[Eqi.tk], r=[cs.tk, sm.tk])
                P.act(lambda e, h=h: e.activation(out=Eki[:, h, 0:w], in_=cs[:, h, 0:w], func=AF.Exp, scale=1.0 / 16, bias=sm[:, 4 + h:5 + h]),
                      w=[Eki.tk], r=[cs.tk, sm.tk])
        if full:
            P.act(lambda e: e.activation(out=Eqn[:, :, 0:w], in_=cs[:, :, 0:w], func=AF.Exp, scale=-1.0 / 16), w=[Eqn.tk], r=[cs.tk])
        pkv4 = pk[:, :].rearrange("p (h c) -> p h c", h=4)[:, :, 0:w]
        P.dve(lambda e: e.tensor_tensor(out=ko[:, :, 0:w], in0=pkv4, in1=Eko[:, :, 0:w], op=ALU.mult), w=[ko.tk], r=[pk.tk, Eko.tk])
        if full:
            pqv4 = pq[:, :].rearrange("p (h c) -> p h c", h=4)[:, :, 0:w]
            sc = GQK ** 0 * (128 ** -0.5)
            P.dve(lambda e: e.scalar_tensor_tensor(out=qt[:, :, 0:w], in0=pqv4, scalar=sc, in1=Eqi[:, :, 0:w], op0=ALU.mult, op1=ALU.mult),
                  w=[qt.tk], r=[pq.tk, Eqi.tk])
            P.dve(lambda e: e.scalar_tensor_tensor(out=qn[:, :, 0:w], in0=pqv4, scalar=sc, in1=Eqn[:, :, 0:w], op0=ALU.mult, op1=ALU.mult),
                  w=[qn.tk], r=[pq.tk, Eqn.tk])
            P.dve(lambda e: e.tensor_tensor(out=kt[:, :, 0:w], in0=pkv4, in1=Eki[:, :, 0:w], op=ALU.mult), w=[kt.tk], r=[pk.tk, Eki.tk])
        for hf in range(2):
            pv = nps()
            for kc in range(8):
                P.pe(lambda e, kc=kc, hf=hf, pv=pv: e.matmul(pv[:w, :], lhsT=hb[:, kc, 0:w], rhs=win[:, kc, 1024 + hf * 512:1024 + (hf + 1) * 512],
                                                          start=(kc == 0), stop=(kc == 7)), w=[pv.tk], r=[win.tk, hb.tk])
            P.act(lambda e, hf=hf, pv=pv: e.copy(out=vbf[:w, hf * 512:(hf + 1) * 512], in_=pv[:w, :]), w=[vbf.tk], r=[pv.tk])
        if full:
            for hf in range(2):
                pr_ = nps()
                for kc in range(8):
                    P.pe(lambda e, kc=kc, hf=hf, pr_=pr_: e.matmul(pr_[:w, :], lhsT=hb[:, kc, 0:w], rhs=win[:, kc, 2048 + hf * 512:2048 + (hf + 1) * 512],
                                                                  start=(kc == 0), stop=(kc == 7)), w=[pr_.tk], r=[win.tk, hb.tk])
                P.act(lambda e, hf=hf, pr_=pr_: e.activation(out=rs[:w, hf * 512:(hf + 1) * 512], in_=pr_[:w, :], func=AF.Silu), w=[rs.tk], r=[pr_.tk])
        pkt = nps()
        for h in range(4):
            P.pe(lambda e, h=h: e.transpose(out=pkt[:w, h * 128:(h + 1) * 128], in_=ko[:, h, 0:w], identity=ident[:, :]),
                 w=[pkt.tk], r=[ko.tk, ident.tk])
        P.act(lambda e: e.copy(out=koT[:w, :], in_=pkt[:w, :]), w=[koT.tk], r=[pkt.tk])
        if full:
            psc = nps()
            for h in range(4):
                P.pe(lambda e, h=h: e.matmul(psc[:w, h * 128:h * 128 + w], lhsT=kt[:, h, 0:w], rhs=qt[:, h, 0:w], start=True, stop=True),
                     w=[psc.tk], r=[kt.tk, qt.tk])
            P.dve(lambda e: e.tensor_tensor(out=scb[:w, :].rearrange("p (h c) -> p h c", h=4)[:, :, 0:w],
                                            in0=psc[:w, :].rearrange("p (h c) -> p h c", h=4)[:, :, 0:w],
                                            in1=cm4[:w, :].rearrange("p (h c) -> p h c", h=4)[:, :, 0:w], op=ALU.mult),
                  w=[scb.tk], r=[psc.tk, cm4.tk])
            po = [nps(), nps()]
            for h in range(4):
                pb = po[h // 2]
                oc = (h % 2) * 256
                P.pe(lambda e, h=h, pb=pb, oc=oc: e.matmul(pb[:w, oc:oc + 256], lhsT=scb[:w, h * 128:h * 128 + w], rhs=vbf[:w, h * 256:(h + 1) * 256],
                                                        start=True, stop=False), w=[pb.tk], r=[scb.tk, vbf.tk])
                P.pe(lambda e, h=h, pb=pb, oc=oc: e.matmul(pb[:w, oc:oc + 256], lhsT=qn[:, h, 0:w], rhs=Sbf[:, h, :],
                                                        start=False, stop=True), w=[pb.tk], r=[qn.tk, Sbf.tks[h]])
        if full:
            o_ = og[ci % 2]
            for h in range(4):
                pb = po[h // 2]
                oc = (h % 2) * 256
                P.dve(lambda e, h=h, pb=pb, oc=oc: e.bn_stats(out=st6[:w, h, :], in_=pb[:w, oc:oc + 256]), w=[st6.tk], r=[pb.tk])
                P.dve(lambda e, h=h: e.bn_aggr(out=mv[:w, h, :], in_=st6[:w, h, :]), w=[mv.tk], r=[st6.tk])
            P.dve(lambda e: e.tensor_scalar(out=rstd[:w, :], in0=mv[:w, :, 1], scalar1=LN_EPS, scalar2=None, op0=ALU.add), w=[rstd.tk], r=[mv.tk])
            P.act(lambda e: e.activation(out=rstd[:w, :], in_=rstd[:w, :], func=AF.Sqrt), w=[rstd.tk], r=[rstd.tk])
            P.dve(lambda e: e.reciprocal(out=rstd[:w, :], in_=rstd[:w, :]), w=[rstd.tk], r=[rstd.tk])
            for h in range(4):
                pb = po[h // 2]
                oc = (h % 2) * 256
                P.dve(lambda e, h=h, pb=pb, oc=oc: e.tensor_scalar(out=o_[:w, h * 256:(h + 1) * 256], in0=pb[:w, oc:oc + 256], scalar1=mv[:w, h, 0:1],
                                                                  scalar2=rstd[:w, h:h + 1], op0=ALU.subtract, op1=ALU.mult),
                      w=[o_.tk], r=[pb.tk, mv.tk, rstd.tk])
        for h in range(4):
            pkv = nps()
            P.pe(lambda e, h=h, pkv=pkv: e.matmul(pkv[:, 0:256], lhsT=koT[:w, h * 128:(h + 1) * 128], rhs=vbf[:w, h * 256:(h + 1) * 256],
                                                 start=True, stop=True), w=[pkv.tk], r=[koT.tk, vbf.tk])
            P.dve(lambda e, h=h, pkv=pkv: e.scalar_tensor_tensor(out=S[:, h, :], in0=S[:, h, :], scalar=sm[:, 12 + h:13 + h], in1=pkv[:, 0:256],
                                                                op0=ALU.mult, op1=ALU.add), w=[S.tks[h]], r=[S.tks[h], pkv.tk, sm.tk])
            if full:
                P.act(lambda e, h=h: e.copy(out=Sbf[:, h, :], in_=S[:, h, :]), w=[Sbf.tks[h]], r=[S.tks[h]])
        if not full:
            return
        P.dve(lambda e: e.tensor_tensor(out=o_[:w, :], in0=o_[:w, :], in1=ng[:w, :], op=ALU.mult), w=[o_.tk], r=[o_.tk, ng.tk])
        P.dve(lambda e: e.tensor_tensor(out=o_[:w, :], in0=o_[:w, :], in1=rs[:w, :], op=ALU.mult), w=[o_.tk], r=[o_.tk, rs.tk])
        pt = [nps(), nps()]
        for jj in range(8):
            P.pe(lambda e, jj=jj: e.transpose(out=pt[jj // 4][:, (jj % 4) * 128:(jj % 4) * 128 + w], in_=o_[:w, jj * 128:(jj + 1) * 128],
                                             identity=ident[:w, :w]), w=[pt[jj // 4].tk], r=[o_.tk, ident.tk])
        for q_ in range(2):
            P.act(lambda e, q_=q_: e.copy(out=ogT[:, 4 * q_:4 * q_ + 4, 0:w], in_=pt[q_][:, :].rearrange("p (j c) -> p j c", j=4)[:, :, 0:w]),
                  w=[ogT.tk], r=[pt[q_].tk])
        rr, ac = Rr[ci % 2], acc[ci % 2]
        P.dma("sp", rr[:w, :], K["R"][row0:row0 + w, :], w=[rr.tk], r=[P.dk("R", row0)])
        for hf in range(2):
            pp = nps()
            for jj in range(8):
                P.pe(lambda e, jj=jj, hf=hf, pp=pp: e.matmul(pp[:w, :], lhsT=ogT[:, jj, 0:w], rhs=wout[:, jj, hf * 512:(hf + 1) * 512],
                                                           start=(jj == 0), stop=(jj == 7)), w=[pp.tk], r=[ogT.tk, wout.tk])
            P.dve(lambda e, hf=hf, pp=pp: e.scalar_tensor_tensor(out=ac[:w, hf * 512:(hf + 1) * 512], in0=rr[:w, hf * 512:(hf + 1) * 512], scalar=ALPHA,
                                                                in1=pp[:w, :], op0=ALU.mult, op1=ALU.add), w=[ac.tk], r=[rr.tk, pp.tk])
        T.run(ac[:w, :], ac.tk, sub)

    for ci, (c, w) in enumerate(gla_chunks()):
        chunk_body(ci, c, w)
        if ci == 0:
            if not full:
                Smeta = P.sb([128, 4, 256], F32)
                P.dve(lambda e: e.tensor_copy(out=Smeta[:, :, :], in_=S[:, :, :]), w=[Smeta.tk], r=list(S.tks))
                for h in range(4):
                    P.dve(lambda e, h=h: e.memset(S[:, h, :], 0.0), w=[S.tks[h]])
                P.dve(lambda e: e.memset(cstot[:, :], 0.0), w=[cstot.tk])
            else:
                for h in range(4):
                    P.dma("sp", S[:, h, :], K["SIN"][:, h * 256:(h + 1) * 256], w=[S.tks[h]], r=[P.dk("SIN")])
                    P.act(lambda e, h=h: e.copy(out=Sbf[:, h, :], in_=S[:, h, :]), w=[Sbf.tks[h]], r=[S.tks[h]])
    if not full:
        summ = P.sb([128, 4 + 1024], F32)
        P.act(lambda e: e.activation(out=summ[:, 0:4], in_=cstot[:, :], func=AF.Exp, scale=-1.0 / 16), w=[summ.tk], r=[cstot.tk])
        P.dve(lambda e: e.tensor_copy(out=summ[:, 4:1028].rearrange("p (h c) -> p h c", h=4), in_=S[:, :, :]), w=[summ.tk], r=list(S.tks) + [summ.tk])
        P.dma("sp", K["XG"][:, :], summ[:, :], w=[P.dk("XG")], r=[summ.tk])
        P.dma("sp", K["SMETA"][:, :], Smeta[:, :, :].rearrange("p h c -> p (h c)"), w=[P.dk("SMETA")], r=[Smeta.tk])
    P.run()


def phase_glafold(G, K):
    P = Phase(G, "glafold")
    Smeta = P.sb([128, 4, 256], F32)
    P.dma("sp", Smeta[:, :, :].rearrange("p h c -> p (h c)"), K["SMETA"][:, :], w=[Smeta.tk], r=[P.dk("SMETA")])
    selw = P.sb([128, 32], F32)
    P.dma("sp", selw[:, :], K["selw"][:, :], w=[selw.tk])
    gs = xch_load(P, K, "XGg", 128, 4 + 1024, q="pool")
    de = P.sb([128, 4], F32)
    for r in range(NCORE):
        P.dve(lambda e, r=r: e.tensor_scalar(out=de[:, :], in0=gs[:, r, 0:4], scalar1=1.0, scalar2=selw[:, r:r + 1], op0=ALU.subtract, op1=ALU.mult),
              w=[de.tk], r=[gs.tk, selw.tk])
        P.dve(lambda e: e.tensor_scalar(out=de[:, :], in0=de[:, :], scalar1=1.0, scalar2=None, op0=ALU.add), w=[de.tk], r=[de.tk])
        for h in range(4):
            P.dve(lambda e, h=h: e.tensor_scalar(out=Smeta[:, h, :], in0=Smeta[:, h, :], scalar1=de[:, h:h + 1], scalar2=None, op0=ALU.mult),
                  w=[Smeta.tk], r=[Smeta.tk, de.tk])
            P.dve(lambda e, h=h, r=r: e.scalar_tensor_tensor(out=Smeta[:, h, :], in0=gs[:, r, 4 + h * 256:4 + (h + 1) * 256], scalar=selw[:, r:r + 1],
                                                            in1=Smeta[:, h, :], op0=ALU.mult, op1=ALU.add), w=[Smeta.tk], r=[Smeta.tk, gs.tk, selw.tk])
    P.dma("sp", K["SIN"][:, :], Smeta[:, :, :].rearrange("p h c -> p (h c)"), w=[P.dk("SIN")], r=[Smeta.tk])
    P.run()


STATE_SHAPES = {
    "R": [NTOK, D], "AU": [2, RG_NT + 1, CT, NCT * RG_TT], "HMETA": [CT, NCT], "SMETA": [128, 1024],
    "XH": [128, 24], "XHg": [NCORE * 128, 24], "XS": [CT, 2 * NCT], "XSg": [NCORE * CT, 2 * NCT],
    "XG": [128, 1028], "XGg": [NCORE * 128, 1028],
}

LAUNCHES = [
    ([], ["prep", "halo"], ["R", "XH"]),
    (["R", "XHg"], ["ht", "rg1:0"], ["AU", "XS", "HMETA"]),
    (["R", "AU", "XSg", "HMETA"], ["ht", "rg2:0:0", "ffn:0:0", "gla1:0:1"], ["R", "XG", "SMETA"]),
    (["R", "XGg", "SMETA"], ["ht", "gla2:0:1", "ffn:0:1", "halo"], ["R", "XH"]),
    (["R", "XHg"], ["ht", "rg1:1"], ["AU", "XS", "HMETA"]),
    (["R", "AU", "XSg", "HMETA"], ["ht", "rg2:1:2", "ffn:1:2", "gla1:1:3"], ["R", "XG", "SMETA"]),
    (["R", "XGg", "SMETA"], ["ht", "gla2:1:3", "ffn:1:3"], []),
]


def run_phase(G, K, spec):
    p = spec.split(":")
    if p[0] == "prep":
        phase_prep(G, K)
    elif p[0] == "halo":
        phase_halo_out(G, K)
    elif p[0] == "ht":
        phase_ht_from_r(G, K)
    elif p[0] == "rg1":
        phase_rg1(G, K, int(p[1]))
    elif p[0] == "rg2":
        phase_rg2(G, K, int(p[1]), int(p[2]))
    elif p[0] == "gla1":
        phase_gla(G, K, int(p[1]), int(p[2]), 1)
    elif p[0] == "gla2":
        phase_glafold(G, K)
        phase_gla(G, K, int(p[1]), int(p[2]), 2)
    elif p[0] == "ffn":
        li = int(p[2])
        phase_ffn(G, K, int(p[1]), li, li % 2 == 1, final=(li == DEPTH - 1))
    elif p[0] == "cc":
        phase_cc(G, K, p[1], p[2])


def new_program(stack):
    nc = bass.Bass("TRN2", target_bir_lowering=False)
    K = KD(nc)
    G = Glob(nc, stack)
    for name, shp in STATE_SHAPES.items():
        K[name] = G.dt(name, shp, F32)
    K["HT"] = G.dt("HT", [9, 128, 8 * 512], BF16)
    K["GT"] = G.dt("GT", [NTOK, NEXP], F32)
    K["SIN"] = G.dt("SIN", [128, 1024], F32)
    return nc, K, G


def copy_state(G, K, names, direction):
    if not names:
        return
    P = Phase(G, "c" + direction)
    for n in names:
        ext = K["_ext_" + direction][n]
        nd = len(STATE_SHAPES[n])
        idx = (slice(None),) * nd
        if direction == "i":
            P.dma("sp", K[n][idx], ext[idx], w=[P.dk(n)])
        else:
            P.dma("sp", ext[idx], K[n][idx], w=[P.dk(n + "_o")], r=[P.dk(n)])
    P.run()


def build_launch(k):
    ins, phases, outs = LAUNCHES[k]
    with ExitStack() as stack:
        nc, K, G = new_program(stack)
        K["_ext_i"] = {n: nc.dram_tensor(n + "_i", list(STATE_SHAPES[n]), F32, kind="ExternalInput").ap() for n in ins}
        K["_ext_o"] = {n: nc.dram_tensor(n + "_o", list(STATE_SHAPES[n]), F32, kind="ExternalOutput").ap() for n in outs}
        if k == len(LAUNCHES) - 1:
            K["out"] = nc.dram_tensor("out", [SEG, D], F32, kind="ExternalOutput").ap()
        copy_state(G, K, ins, "i")
        for spec in phases:
            run_phase(G, K, spec)
        copy_state(G, K, outs, "o")
    return nc, [k_ for k_ in K.keys() if k_ in EXT_SHAPES]


def build_fused():
    with ExitStack() as stack:
        nc, K, G = new_program(stack)
        K["out"] = nc.dram_tensor("out", [SEG, D], F32, kind="ExternalOutput").ap()
        seq = ["prep", "halo", "cc:XH:XHg", "rg1:0", "cc:XS:XSg", "rg2:0:0", "ffn:0:0", "gla1:0:1", "cc:XG:XGg",
               "gla2:0:1", "ffn:0:1", "halo", "cc:XH:XHg", "rg1:1", "cc:XS:XSg", "rg2:1:2", "ffn:1:2", "gla1:1:3", "cc:XG:XGg",
               "gla2:1:3", "ffn:1:3"]
        for spec in seq:
            run_phase(G, K, spec)
    return nc, [k_ for k_ in K.keys() if k_ in EXT_SHAPES]


def host_inputs(inp):
    f = lambda a: np.ascontiguousarray(np.asarray(a, dtype=np.float32))
    x = f(inp["x"])
    com = {}
    com["meta"] = f(inp["meta_tokens"])
    com["ident"] = np.eye(128, dtype=np.float32)
    com["ln_gain"] = f(inp["ln_gain"]).reshape(DEPTH * 2, D)
    com["ln_bias"] = f(inp["ln_bias"]).reshape(DEPTH * 2, D)
    com["rg_w_in"] = f(inp["rg_w_in"])
    wg = f(inp["rg_w_gates"])
    com["rg_wg"] = np.ascontiguousarray(wg.reshape(2, 2, 8, 2, CT, 168).transpose(0, 4, 1, 2, 3, 5).reshape(2, CT, -1))
    cw = f(inp["rg_conv_w"])
    prm = np.concatenate([cw, f(inp["rg_conv_b"])[:, None], f(inp["rg_b_gates"]), f(inp["rg_lambda"])[:, None]], axis=1)
    com["rg_prm"] = np.ascontiguousarray(prm.reshape(2, 8, NCT, CT).transpose(0, 3, 1, 2).reshape(2, CT, 8 * NCT))
    com["rg_w_out"] = f(inp["rg_w_out"])
    com["cmask"] = np.triu(np.ones((128, 128), np.float32))
    com["gla_w_in"] = f(inp["gla_w_in"])
    com["gla_w_gate_up"] = f(inp["gla_w_gate_up"])
    com["gla_b_gate"] = np.ascontiguousarray(f(inp["gla_b_gate"]).reshape(2, 4, 128).transpose(0, 2, 1))
    com["gla_norm_g"] = f(inp["gla_norm_g"]).reshape(2, GVD)
    com["gla_w_out"] = f(inp["gla_w_out"])
    com["ffn_w_in"] = f(inp["ffn_w_in"])
    com["ffn_w_out"] = f(inp["ffn_w_out"])
    for k in ("moe_router", "moe_w_in", "moe_w_out"):
        if k in inp:
            com[k] = f(inp[k])
    maps = []
    for c in range(NCORE):
        b, s = c // 4, c % 4
        m = dict(com)
        m["x"] = np.ascontiguousarray(x[b, s * SEG:(s + 1) * SEG])
        sel = np.zeros((128, 32), np.float32)
        for r in range(NCORE):
            if r // 4 == b and r % 4 < s:
                sel[:, r] = 1.0
        if s == 0:
            sel[:, 16] = 1.0
        else:
            sel[:, 8 + c - 1] = 1.0
        m["selw"] = sel
        maps.append(m)
    return maps


FUSED = False


def kernel(**inp):
    base = host_inputs(inp)
    out = np.zeros((2, 4 * SEG, D), np.float32)
    if FUSED:
        nc, used = build_fused()
        maps = [{k: m[k] for k in used} for m in base]
        res = run_bass_kernel_spmd(nc, maps, core_ids=list(range(NCORE)))
        for c in range(NCORE):
            out[c // 4, (c % 4) * SEG:(c % 4 + 1) * SEG] = res.results[c]["out"]
        return out
    state = [dict() for _ in range(NCORE)]
    for k, (ins, phases, outs) in enumerate(LAUNCHES):
        nc, used = build_launch(k)
        maps = []
        for c in range(NCORE):
            m = {k_: base[c][k_] for k_ in used}
            for n in ins:
                m[n + "_i"] = state[c][n]
            maps.append(m)
        res = run_bass_kernel_spmd(nc, maps, core_ids=list(range(NCORE)))
        for c in range(NCORE):
            for n in outs:
                state[c][n] = np.ascontiguousarray(res.results[c][n + "_o"])
        for n in outs:
            if n in ("XH", "XS", "XG"):
                g = np.ascontiguousarray(np.concatenate([state[c][n] for c in range(NCORE)], axis=0))
                for c in range(NCORE):
                    state[c][n + "g"] = g
        if k == len(LAUNCHES) - 1:
            for c in range(NCORE):
                out[c // 4, (c % 4) * SEG:(c % 4 + 1) * SEG] = res.results[c]["out"]
    return out
```
